# Optimizing a Trainium2 kernel written in Bass

```python
import jax, jax.numpy as jnp
from jax import lax
import numpy as np

D_MODEL = 1024
BATCH = 4
SEQ = 4096
DEPTH = 2
DEC_BATCH = 32
DEC_SEQ = 4
PAST_LEN = 8192
PAGE_SIZE = 128

SB_HEADS = 8
HEAD_DIM = 64
SB_WIDTH = SB_HEADS * HEAD_DIM
D_CONV = D_MODEL // 2
CONV_GROUPS = 8
CONV_K = 3
D_FF = 2816
N_EXPERTS = 8
TOP_K = 2
D_FF_EXPERT = 3584
Q_BLOCK = 128
N_DENSE = (DEPTH + 1) // 2
N_MOE = DEPTH // 2
RMS_EPS = 1e-6
SB_BIAS_INIT = -6.0
IN_SPLITS = [SB_WIDTH, SB_WIDTH, SB_WIDTH, D_CONV, D_CONV, D_CONV, D_MODEL, D_MODEL]
D_IN = sum(IN_SPLITS)
SB_SCALE = HEAD_DIM ** -0.5

kernel_name = "stickbreak_shortconv_gated_hybrid_step"


def rmsnorm(x, g):
    xf = x.astype(jnp.float32)
    y = xf * lax.rsqrt(jnp.mean(xf * xf, axis=-1, keepdims=True) + RMS_EPS)
    return (y * g.astype(jnp.float32)).astype(x.dtype)


def sb_weights(z, mask):
    log_one_minus = jnp.where(mask, -jax.nn.softplus(z), 0.0)
    excl = lax.cumsum(log_one_minus, axis=z.ndim - 1, reverse=True) - log_one_minus
    return jnp.where(mask, jnp.exp(jax.nn.log_sigmoid(z) + excl), 0.0)


def sb_prompt(q, k, v, bias):
    b, s, h, dh = q.shape
    nb = s // Q_BLOCK
    qb = q.reshape(b, nb, Q_BLOCK, h, dh).transpose(1, 0, 2, 3, 4)
    kpos = jnp.arange(s)
    bias_f = bias.astype(jnp.float32)[None, :, None, None]

    def block(args):
        qi, start = args
        z = jnp.einsum('bqhd,bkhd->bhqk', qi, k, preferred_element_type=jnp.float32) * SB_SCALE + bias_f
        qpos = start + jnp.arange(Q_BLOCK)
        mask = kpos[None, :] < qpos[:, None]
        w = sb_weights(z, mask)
        return jnp.einsum('bhqk,bkhd->bqhd', w.astype(v.dtype), v)

    o = lax.map(block, (qb, jnp.arange(nb) * Q_BLOCK))
    return o.transpose(1, 0, 2, 3, 4).reshape(b, s, h * dh)


def sb_sample(q, k_new, v_new, bias, k_past, v_past):
    b, t, h, dh = q.shape
    p = k_past.shape[1]
    bias_f = bias.astype(jnp.float32)[None, :, None, None]
    zp = jnp.einsum('bqhd,bkhd->bhqk', q, k_past, preferred_element_type=jnp.float32) * SB_SCALE
    zn = jnp.einsum('bqhd,bkhd->bhqk', q, k_new, preferred_element_type=jnp.float32) * SB_SCALE
    z = jnp.concatenate([zp, zn], axis=-1) + bias_f
    ar = jnp.arange(t)
    mask = jnp.concatenate([jnp.ones((t, p), dtype=bool), ar[None, :] < ar[:, None]], axis=1)
    w = sb_weights(z, mask).astype(v_new.dtype)
    o = (jnp.einsum('bhqk,bkhd->bqhd', w[..., :p], v_past)
         + jnp.einsum('bhqk,bkhd->bqhd', w[..., p:], v_new))
    return o.reshape(b, t, h * dh)


def short_conv(u, buf, w):
    t = u.shape[1]
    full = jnp.concatenate([buf, u], axis=1)
    y = w[0] * full[:, 0:t]
    for i in range(1, CONV_K):
        y = y + w[i] * full[:, i:i + t]
    return y, full[:, t:]


def mixer(xn, w_in, sb_bias, w_a, conv_w, w_b, w_o, attend, conv_buf):
    b, t, _ = xn.shape
    p = xn @ w_in
    q, k, v, bg, cg, hh, ga, gb = jnp.split(p, list(np.cumsum(IN_SPLITS)[:-1]), axis=-1)
    q = q.reshape(b, t, SB_HEADS, HEAD_DIM)
    k = k.reshape(b, t, SB_HEADS, HEAD_DIM)
    v = v.reshape(b, t, SB_HEADS, HEAD_DIM)
    ya = attend(q, k, v, sb_bias) @ w_a
    cv, new_buf = short_conv(cg * hh, conv_buf, conv_w)
    yb = (bg * cv) @ w_b
    y = (jax.nn.sigmoid(ga) * ya + jax.nn.sigmoid(gb) * yb) @ w_o
    return y, k, v, new_buf


def swiglu(x, wg, wu, wd):
    return (jax.nn.silu(x @ wg) * (x @ wu)) @ wd


def moe(x, router, wg, wu, wd):
    shp = x.shape
    xf = x.reshape(-1, shp[-1])
    logits = (xf @ router).astype(jnp.float32)
    topv, topi = lax.top_k(logits, TOP_K)
    gates = jax.nn.softmax(topv, axis=-1)
    comb = jnp.sum(jax.nn.one_hot(topi, N_EXPERTS, dtype=jnp.float32) * gates[..., None], axis=-2).astype(x.dtype)
    out = comb[:, 0:1] * swiglu(xf, wg[0], wu[0], wd[0])
    for e in range(1, N_EXPERTS):
        out = out + comb[:, e:e + 1] * swiglu(xf, wg[e], wu[e], wd[e])
    return out.reshape(shp)


def setup_inputs(seed: int = 0) -> dict:
    key = jax.random.key(seed)
    ks = jax.random.split(key, 24)
    n_pages = PAST_LEN // PAGE_SIZE
    n_used = DEC_BATCH * n_pages
    n_pool = (n_used * 5) // 4
    nrm = jax.random.normal
    f32 = jnp.float32
    perm = jax.random.permutation(ks[5], n_pool)[:n_used]
    return {
        "x_prompt": nrm(ks[0], (BATCH, SEQ, D_MODEL), f32),
        "x_sample": nrm(ks[1], (DEC_BATCH, DEC_SEQ, D_MODEL), f32),
        "cache_k": nrm(ks[2], (DEPTH, n_pool, PAGE_SIZE, SB_HEADS, HEAD_DIM), f32),
        "cache_v": nrm(ks[3], (DEPTH, n_pool, PAGE_SIZE, SB_HEADS, HEAD_DIM), f32),
        "state_conv": nrm(ks[4], (DEPTH, DEC_BATCH, CONV_K - 1, D_CONV), f32),
        "page_table": perm.reshape(DEC_BATCH, n_pages).astype(jnp.int32),
        "norm_mix": 1.0 + 0.02 * nrm(ks[6], (DEPTH, D_MODEL), f32),
        "w_in": nrm(ks[7], (DEPTH, D_MODEL, D_IN), f32) * D_MODEL ** -0.5,
        "sb_bias": SB_BIAS_INIT + 0.1 * nrm(ks[21], (DEPTH, SB_HEADS), f32),
        "w_a": nrm(ks[8], (DEPTH, SB_WIDTH, D_MODEL), f32) * SB_WIDTH ** -0.5,
        "conv_w": nrm(ks[9], (DEPTH, CONV_K, D_CONV), f32) * CONV_K ** -0.5,
        "w_b": nrm(ks[10], (DEPTH, D_CONV, D_MODEL), f32) * D_CONV ** -0.5,
        "w_o": nrm(ks[11], (DEPTH, D_MODEL, D_MODEL), f32) * D_MODEL ** -0.5,
        "norm_ffn": 1.0 + 0.02 * nrm(ks[12], (DEPTH, D_MODEL), f32),
        "ffn_wg": nrm(ks[13], (N_DENSE, D_MODEL, D_FF), f32) * D_MODEL ** -0.5,
        "ffn_wu": nrm(ks[14], (N_DENSE, D_MODEL, D_FF), f32) * D_MODEL ** -0.5,
        "ffn_wd": nrm(ks[15], (N_DENSE, D_FF, D_MODEL), f32) * D_FF ** -0.5,
        "router": nrm(ks[16], (N_MOE, D_MODEL, N_EXPERTS), f32) * D_MODEL ** -0.5,
        "moe_wg": nrm(ks[17], (N_MOE, N_EXPERTS, D_MODEL, D_FF_EXPERT), f32) * D_MODEL ** -0.5,
        "moe_wu": nrm(ks[18], (N_MOE, N_EXPERTS, D_MODEL, D_FF_EXPERT), f32) * D_MODEL ** -0.5,
        "moe_wd": nrm(ks[19], (N_MOE, N_EXPERTS, D_FF_EXPERT, D_MODEL), f32) * D_FF_EXPERT ** -0.5,
        "norm_final": 1.0 + 0.02 * nrm(ks[20], (D_MODEL,), f32),
    }


def reference(x_prompt, x_sample, cache_k, cache_v, state_conv, page_table,
              norm_mix, w_in, sb_bias, w_a, conv_w, w_b, w_o, norm_ffn,
              ffn_wg, ffn_wu, ffn_wd, router, moe_wg, moe_wu, moe_wd, norm_final):
    db, n_pages = page_table.shape
    past = n_pages * PAGE_SIZE
    xp, xs = x_prompt, x_sample
    kp_l, vp_l, cp_l, ks_l, vs_l, cs_l = [], [], [], [], [], []
    for l in range(DEPTH):
        zero_buf = jnp.zeros((xp.shape[0], CONV_K - 1, D_CONV), xp.dtype)
        hp, kp, vp, bp = mixer(rmsnorm(xp, norm_mix[l]), w_in[l], sb_bias[l], w_a[l], conv_w[l], w_b[l], w_o[l],
                               sb_prompt, zero_buf)
        k_past = cache_k[l][page_table].reshape(db, past, SB_HEADS, HEAD_DIM)
        v_past = cache_v[l][page_table].reshape(db, past, SB_HEADS, HEAD_DIM)
        hs, ksn, vsn, bs = mixer(rmsnorm(xs, norm_mix[l]), w_in[l], sb_bias[l], w_a[l], conv_w[l], w_b[l], w_o[l],
                                 lambda q, k, v, bias: sb_sample(q, k, v, bias, k_past, v_past), state_conv[l])
        xp = xp + hp
        xs = xs + hs
        fi = l // 2
        if l % 2 == 0:
            xp = xp + swiglu(rmsnorm(xp, norm_ffn[l]), ffn_wg[fi], ffn_wu[fi], ffn_wd[fi])
            xs = xs + swiglu(rmsnorm(xs, norm_ffn[l]), ffn_wg[fi], ffn_wu[fi], ffn_wd[fi])
        else:
            xp = xp + moe(rmsnorm(xp, norm_ffn[l]), router[fi], moe_wg[fi], moe_wu[fi], moe_wd[fi])
            xs = xs + moe(rmsnorm(xs, norm_ffn[l]), router[fi], moe_wg[fi], moe_wu[fi], moe_wd[fi])
        kp_l.append(kp); vp_l.append(vp); cp_l.append(bp)
        ks_l.append(ksn); vs_l.append(vsn); cs_l.append(bs)
    y_prompt = rmsnorm(xp, norm_final)
    y_sample = rmsnorm(xs, norm_final)
    new_k_prompt = jnp.stack(kp_l)
    new_v_prompt = jnp.stack(vp_l)
    new_conv_prompt = jnp.stack(cp_l)
    new_k_sample = jnp.stack(ks_l)
    new_v_sample = jnp.stack(vs_l)
    new_conv_sample = jnp.stack(cs_l)
    return (y_prompt, y_sample, new_k_prompt, new_v_prompt, new_conv_prompt, new_k_sample, new_v_sample, new_conv_sample)
```

```python
import numpy as np
import concourse.bass as bass
import concourse.mybir as mybir
from concourse.bass_utils import run_bass_kernel_spmd

F32 = mybir.dt.float32
BF16 = mybir.dt.bfloat16
I32 = mybir.dt.int32
ALU = mybir.AluOpType
AF = mybir.ActivationFunctionType
AX = mybir.AxisListType

D = 1024
DEPTH = 2
NPB = 16
NP = NPB * 128
NS = 128
NT = NP + NS
KC = 8
D_IN = 5120
D_FF = 2816
D_FFE = 3584
NE = 8
N_POOL = 2560
N_PAGES = 64
DB = 32
GROUPS = [(0, 512), (512, 512), (1024, 512), (1536, 512), (2048, 128)]
EPS = 1e-6
C_Q, C_K, C_V, C_B, C_C, C_H, C_GA, C_GB = 0, 512, 1024, 1536, 2048, 2560, 3072, 4096
V_GM, V_GF, V_GFIN, V_CW, V_SBB, V_SBBC, V_SEL, V_EPS = 0, 16, 32, 40, 64, 80, 82, 84
NVEC = 85


SEM_LIMIT = 12000


class Ev:
    __slots__ = ("sem", "val", "clock")

    def __init__(self, sem, val, clock):
        self.sem = sem
        self.val = val
        self.clock = clock


class Buf:
    __slots__ = ("name", "last_w", "reads")

    def __init__(self, name=""):
        self.name = name
        self.last_w = None
        self.reads = {}


class Eng:
    def __init__(self, name, obj, sem, is_pe=False):
        self.name = name
        self.obj = obj
        self.sem = sem
        self.count = 0
        self.clock = {}
        self.is_pe = is_pe
        self.own = {id(sem)}
        self.n_wait = 0
        self.n_ins = 0


class Sched:
    def __init__(self, nc, n_dma_sems=48):
        self.nc = nc
        self.ctx = []
        self.engs = {}
        for name, obj, ispe in (("pe", nc.tensor, True), ("act", nc.scalar, False),
                                ("dve", nc.vector, False), ("pool", nc.gpsimd, False),
                                ("sp", nc.sync, False)):
            cm = nc.semaphore("sem_" + name)
            sem = cm.__enter__()
            self.ctx.append(cm)
            self.engs[name] = Eng(name, obj, sem, ispe)
        self.spare = []
        for i in range(10):
            cm = nc.semaphore("spare%d" % i)
            self.spare.append(cm.__enter__())
            self.ctx.append(cm)
        self.dma_sems = []
        for i in range(n_dma_sems):
            cm = nc.semaphore("dsem%d" % i)
            sem = cm.__enter__()
            self.ctx.append(cm)
            self.dma_sems.append([sem, 0, None])
        self.dma_rr = 0

    def _need(self, eng, ev):
        if ev is None:
            return
        key = id(ev.sem)
        if eng.clock.get(key, 0) >= ev.val:
            return
        if eng.is_pe and id(ev.sem) in eng.own:
            eng.clock[key] = ev.val
            return
        eng.obj.wait_ge(ev.sem, ev.val)
        eng.n_wait += 1
        ck = eng.clock
        for k, v in ev.clock.items():
            if ck.get(k, 0) < v:
                ck[k] = v
        if ck.get(key, 0) < ev.val:
            ck[key] = ev.val

    def _deps(self, eng, reads, writes):
        for b in reads:
            self._need(eng, b.last_w)
        for b in writes:
            self._need(eng, b.last_w)
            for ev in b.reads.values():
                self._need(eng, ev)

    def _commit(self, ev, reads, writes):
        for b in reads:
            b.reads[id(ev.sem)] = ev
        for b in writes:
            b.last_w = ev
            b.reads = {}

    def op(self, engname, fn, reads=(), writes=()):
        eng = self.engs[engname]
        self._deps(eng, reads, writes)
        if eng.count >= SEM_LIMIT:
            eng.sem = self.spare.pop()
            eng.own.add(id(eng.sem))
            eng.count = 0
        ins = fn(eng.obj)
        eng.count += 1
        eng.n_ins += 1
        ins.then_inc(eng.sem, 1)
        clock = dict(eng.clock)
        clock[id(eng.sem)] = eng.count
        if eng.is_pe:
            eng.clock[id(eng.sem)] = eng.count
        ev = Ev(eng.sem, eng.count, clock)
        self._commit(ev, reads, writes)
        return ev

    def dma(self, engname, fn, reads=(), writes=()):
        eng = self.engs[engname]
        self._deps(eng, reads, writes)
        slot = self.dma_sems[self.dma_rr]
        self.dma_rr = (self.dma_rr + 1) % len(self.dma_sems)
        self._need(eng, slot[2])
        res = fn(eng.obj)
        if not isinstance(res, (list, tuple)):
            res = [res]
        for ins in res:
            ins.then_inc(slot[0], 16)
            slot[1] += 16
        eng.n_ins += len(res)
        clock = dict(eng.clock)
        clock[id(slot[0])] = slot[1]
        ev = Ev(slot[0], slot[1], clock)
        slot[2] = ev
        self._commit(ev, reads, writes)
        return ev

    def alias(self, newbufs, oldbufs):
        for nb in newbufs:
            for ob in oldbufs:
                if ob.last_w is not None:
                    nb.reads[("w", id(ob), id(ob.last_w.sem))] = ob.last_w
                for k, ev in ob.reads.items():
                    nb.reads[("r", id(ob), k)] = ev

    def finish(self):
        sp = self.engs["sp"]
        self.final_events = []
        for slot in self.dma_sems:
            self._need(sp, slot[2])
        for e in self.engs.values():
            if e.count:
                self._need(sp, Ev(e.sem, e.count, {}))

    def stats(self):
        return {k: (e.n_ins, e.n_wait) for k, e in self.engs.items()}


class Ring:
    def __init__(self, nc, name, n, shape, dt):
        self.tiles = []
        self.bufs = []
        for i in range(n):
            self.tiles.append(nc.sbuf_tensor("%s%d" % (name, i), shape, dt).__enter__())
            self.bufs.append(Buf("%s%d" % (name, i)))
        self.i = 0

    def get(self):
        t, b = self.tiles[self.i], self.bufs[self.i]
        self.i = (self.i + 1) % len(self.tiles)
        return t, b


def build_program(stop_after=None, skip_sample=False):
    nc = bass.Bass("TRN2", target_bir_lowering=False)
    S = Sched(nc)
    SPE = mybir.EngineType.SP

    declared = []

    def din(name, shape, dt=F32, use=True):
        if not use:
            return None
        declared.append(name)
        return nc.dram_tensor(name, list(shape), dt, kind="ExternalInput").ap()

    def dout(name, shape, dt=F32):
        return nc.dram_tensor(name, list(shape), dt, kind="ExternalOutput").ap()

    xT_d = din("xT", [D, NT])
    w_in_d = din("w_in", [DEPTH, D, D_IN])
    w_a_d = din("w_a", [DEPTH, 512, D])
    w_b_d = din("w_b", [DEPTH, 512, D])
    w_o_d = din("w_o", [DEPTH, D, D])
    wqkv_d = din("wqkv", [DEPTH, D, 192])
    fwg_d = din("ffn_wg", [1, D, D_FF])
    fwu_d = din("ffn_wu", [1, D, D_FF])
    fwd_d = din("ffn_wd", [1, D_FF, D])
    rt_d = din("router", [128, KC, NE])
    mwg_d = din("moe_wg", [1, NE, D, D_FFE], use=stop_after not in ("pass1", "prompt_mixer", "mixer", "layer0", "l1pass1"))
    mwu_d = din("moe_wu", [1, NE, D, D_FFE], use=stop_after not in ("pass1", "prompt_mixer", "mixer", "layer0", "l1pass1"))
    mwd_d = din("moe_wd", [1, NE, D_FFE, D], use=stop_after not in ("pass1", "prompt_mixer", "mixer", "layer0", "l1pass1"))
    vec_d = din("vec", [128, NVEC])
    masks_d = din("masks", [128, 8, 512])
    cst_d = din("cst", [128, 7, 128])
    pt_d = din("ptT2", [128, DB // 2], I32)
    ckq_d = [din("ckq%d" % i, [N_POOL, 2048], use=not skip_sample) for i in range(DEPTH * 4)]
    cvq_d = [din("cvq%d" % i, [N_POOL, 2048], use=not skip_sample) for i in range(DEPTH * 4)]
    scT_d = din("scT", [DEPTH, 128, 4, 2, DB])
    o_y = dout("o_y", [D, NT])
    o_k = dout("o_k", [DEPTH, 512, NT])
    o_v = dout("o_v", [DEPTH, NT, 512])
    o_tail = dout("o_tail", [DEPTH, 128, 4, NPB, 2])
    o_convs = dout("o_convs", [DEPTH, 128, 4, 2, DB])
    DBG = stop_after == "l1pass1" and not skip_sample
    if DBG:
        d_osp = dout("d_osp", [64, NS])
        d_z = dout("d_z", [128, 512])
        d_x = dout("d_x", [128, 512])
        d_w = dout("d_w", [128, 512])
        d_g = dout("d_g", [128, 8])
        d_tn = dout("d_tn", [128, 128])
        d_new = dout("d_new", [64, NS])
    xk_d = nc.dram_tensor("xk", [512, NP], BF16)
    xv_d = nc.dram_tensor("xv", [NP, 512], BF16)
    xt_d = nc.dram_tensor("xt", [512, NPB * 2], F32)
    gk_d = nc.dram_tensor("gk", [1024, NP], BF16)
    gv_d = nc.dram_tensor("gv", [2 * NP, 512], BF16)
    gt_d = nc.dram_tensor("gt", [1024, NPB * 2], F32)
    xs_d = nc.dram_tensor("xs", [64, NS], BF16)
    gs_d = nc.dram_tensor("gs", [512, NS], BF16)
    qsd_d = nc.dram_tensor("qsd", [NS, 64], F32)
    B_qsd = Buf("qsd")
    B_xk, B_xv, B_xt, B_gk, B_gv, B_gt, B_xs, B_gs = [Buf(n) for n in
                                                      ("xk", "xv", "xt", "gk", "gv", "gt", "xs", "gs")]

    def sb(name, shape, dt):
        return nc.sbuf_tensor("s_" + name, list(shape), dt).__enter__()

    x = sb("x", [128, KC, NT], F32)
    B_x = [[Buf("x%d_%d" % (k, g)) for g in range(len(GROUPS))] for k in range(KC)]
    arena = sb("arena", [128, 18432], BF16)
    uT = sb("uT", [128, 4, NPB, 130], BF16)
    B_uT = Buf("uT")
    vec = sb("vec", [128, NVEC], F32)
    B_vec = Buf("vec")
    masks = sb("masks", [128, 8, 512], BF16)
    B_masks = Buf("masks")
    tri = sb("tri", [128, 4, 128], BF16)
    B_tri = Buf("tri")
    cf = sb("cf", [128, 3, 128], F32)
    rt = sb("rt", [128, KC, NE], F32)
    B_const = Buf("const")
    ptT2 = sb("ptT2", [128, DB // 2], I32)
    B_pt = Buf("pt")
    zeros = sb("zeros", [128, 512], BF16)
    B_zeros = Buf("zeros")
    onesf = sb("onesf", [128, 128], F32)
    tails = sb("tails", [128, 4, NPB, 2], F32)
    B_tails = Buf("tails")
    th = sb("th", [128, 2, 4, NPB, 2], F32)
    B_th = Buf("th")
    comb = sb("comb", [128, NT // 128, NE], F32)
    B_comb = Buf("comb")
    comb_b = sb("comb_b", [128, NT], BF16)
    B_combb = Buf("comb_b")
    xn_full = arena[:, 0:KC * NT].rearrange("p (k n) -> p k n", k=KC)
    B_xnf = [Buf("xnf%d" % g) for g in range(len(GROUPS))]
    xn_g = [arena[:, i * 4096:(i + 1) * 4096].rearrange("p (k n) -> p k n", k=KC) for i in range(2)]
    B_xng = [Buf("xng0"), Buf("xng1")]
    QT_g = arena[:, 8192:10240].rearrange("p (k n) -> p k n", k=4)
    B_QT = Buf("QT")
    attnT = arena[:, 10240:12288].rearrange("p (k n) -> p k n", k=4)
    B_attnT = Buf("attnT")
    bcT = arena[:, 12288:14336].rearrange("p (k n) -> p k n", k=4)
    B_bcT = Buf("bcT")
    mT = arena[:, 14336:18432].rearrange("p (k n) -> p k n", k=8)
    B_mT = Buf("mT")
    arena_mix = B_xng + [B_QT, B_attnT, B_bcT, B_mT]

    wring = Ring(nc, "wb", 4, [128, 4096], BF16)
    tf = Ring(nc, "tf", 6, [128, 512], F32)
    tb = Ring(nc, "tb", 6, [128, 512], BF16)
    kvring = Ring(nc, "kv", 4, [128, 3, 128], BF16)
    PS = [nc.psum_tensor("ps%d" % i, [128, 512], F32).__enter__() for i in range(8)]
    B_PS = [Buf("ps%d" % i) for i in range(8)]
    ps_rr = [0]

    def psum():
        i = ps_rr[0]
        ps_rr[0] = (i + 1) % 6
        return PS[i], B_PS[i]

    qsT = sb("qsT", [64, NS], F32)
    ksT = sb("ksT", [64, NS], F32)
    vnt = sb("vnt", [128, 64], F32)
    tnt = sb("tnt", [128, 128], F32)
    gtn = sb("gtn", [128, 4], F32)
    tot4 = sb("tot4", [128, 4], F32)
    qtm = sb("qtm", [128, 64], F32)
    qrep = sb("qrep", [128, 4, 64], F32)
    B_tot4, B_qtm, B_qrep = Buf("tot4"), Buf("qtm"), Buf("qrep")
    B_qs, B_ks, B_vn, B_tn, B_gtn = Buf("qs"), Buf("ks"), Buf("vn"), Buf("tn"), Buf("gtn")
    wqkv = sb("wqkv", [128, KC, 192], BF16)
    B_wqkv = Buf("wqkv")
    full = sb("full", [128, 4, 6, DB], F32)
    B_full = Buf("full")
    ospT = sb("ospT", [64, NS], BF16)
    B_osp = Buf("osp")
    print("sbuf bytes remaining:", nc.sbuf_bytes_remaining)

    def vcol(c):
        return vec[:, c:c + 1]

    def load_w(src_ap, k, n, eng="pool"):
        t, b = wring.get()
        view = t[:, 0:k * n].rearrange("p (k n) -> p k n", k=k)
        S.dma(eng, lambda e: e.dma_start(out=view, in_=src_ap.rearrange("(k p) n -> p k n", p=128)),
              writes=[b])
        return view, b

    def mm(ps_ap, lhsT, rhs, start, stop, reads, wbuf):
        S.op("pe", lambda e: e.matmul(ps_ap, lhsT=lhsT, rhs=rhs, start=start, stop=stop),
             reads=reads, writes=[wbuf])

    def act(out, in_, func, reads, writes, bias=None, scale=None):
        kw = {}
        if bias is not None:
            kw["bias"] = bias
        if scale is not None:
            kw["scale"] = scale
        S.op("act", lambda e: e.activation(out=out, in_=in_, func=func, **kw), reads=reads, writes=writes)

    def tt(eng, out, in0, in1, op, reads, writes):
        S.op(eng, lambda e: e.tensor_tensor(out=out, in0=in0, in1=in1, op=op), reads=reads, writes=writes)

    def ts(eng, out, in0, s1, op0, reads, writes, s2=None, op1=None):
        if op1 is None:
            S.op(eng, lambda e: e.tensor_scalar(out=out, in0=in0, scalar1=s1, scalar2=None, op0=op0),
                 reads=reads, writes=writes)
        else:
            S.op(eng, lambda e: e.tensor_scalar(out=out, in0=in0, scalar1=s1, scalar2=s2, op0=op0, op1=op1),
                 reads=reads, writes=writes)

    def stt(eng, out, in0, scalar, in1, op0, op1, reads, writes):
        S.op(eng, lambda e: e.scalar_tensor_tensor(out=out, in0=in0, scalar=scalar, in1=in1, op0=op0, op1=op1),
             reads=reads, writes=writes)

    def cp(eng, out, in_, reads, writes):
        if eng == "act":
            S.op("act", lambda e: e.copy(out=out, in_=in_), reads=reads, writes=writes)
        else:
            S.op(eng, lambda e: e.tensor_copy(out=out, in_=in_), reads=reads, writes=writes)

    rkeep = sb("rkeep", [128, 512], F32)
    B_rkeep = Buf("rkeep")

    def rstd_group(gi, keep=False):
        t0, nt = GROUPS[gi]
        ps, bps = psum()
        for kc in range(KC):
            sq, bsq = tb.get()
            act(sq[:, :nt], x[:, kc, t0:t0 + nt], AF.Square, [B_x[kc][gi]], [bsq])
            mm(ps[:, :nt], tri[:, 2, :], sq[:, :nt], kc == 0, kc == KC - 1, [bsq, B_tri], bps)
        lnv, bl = tf.get()
        act(lnv[:, :nt], ps[:, :nt], AF.Ln, [bps, B_vec], [bl], bias=vcol(V_EPS), scale=1.0 / D)
        rstd, br = (rkeep, B_rkeep) if keep else tf.get()
        act(rstd[:, :nt], lnv[:, :nt], AF.Exp, [bl], [br], scale=-0.5)
        return rstd, br

    def rmsnorm_group(gi, gcol, out_view, out_buf):
        t0, nt = GROUPS[gi]
        rstd, br = rstd_group(gi)
        for kc in range(KC):
            stt("dve", out_view[:, kc, :nt], x[:, kc, t0:t0 + nt], vcol(gcol + kc), rstd[:, :nt],
                ALU.mult, ALU.mult, [B_x[kc][gi], br, B_vec], [out_buf])
        return rstd, br

    def proj(wv_, bw, rhs_view, brhs, nt, nsub=4, ksz=KC):
        for sub in range(nsub):
            ps, bps = psum()
            for kc in range(ksz):
                mm(ps[:, :nt], wv_[:, kc, sub * 128:(sub + 1) * 128], rhs_view[:, kc, :nt], kc == 0, kc == ksz - 1,
                   [bw, brhs], bps)
            yield sub, ps, bps

    S.dma("sp", lambda e: e.dma_start(out=vec[:], in_=vec_d), writes=[B_vec])
    for kc in range(KC):
        for gi, (t0, nt) in enumerate(GROUPS):
            S.dma("sp", lambda e, kc=kc, t0=t0, nt=nt: e.dma_start(
                out=x[:, kc, t0:t0 + nt], in_=xT_d[kc * 128:(kc + 1) * 128, t0:t0 + nt]), writes=[B_x[kc][gi]])
    S.dma("pool", lambda e: e.dma_start(out=masks[:], in_=masks_d), writes=[B_masks])
    S.dma("pool", lambda e: e.dma_start(out=tri[:], in_=cst_d[:, 0:4, :]), writes=[B_tri])
    S.dma("sp", lambda e: [e.dma_start(out=cf[:], in_=cst_d[:, 4:7, :]), e.dma_start(out=rt[:], in_=rt_d)],
          writes=[B_const])
    S.dma("sp", lambda e: e.dma_start(out=ptT2[:], in_=pt_d), writes=[B_pt])
    S.op("dve", lambda e: e.memset(zeros[:], 0.0), writes=[B_zeros])
    S.op("dve", lambda e: e.memset(onesf[:], 1.0), writes=[B_zeros])
    for i in range(4):
        S.op("pool", lambda e, i=i: e.memset(kvring.tiles[i][:], 0.0), writes=[kvring.bufs[i]])
    S.op("pool", lambda e: e.memset(uT[:], 0.0), writes=[B_uT])
    maskn2 = cf[:, 0, :]
    ident = cf[:, 1, :]
    u2 = cf[:, 2, :]
    pairs = [[0, 1], [2, 3], [4, 5], [6, 7]]

    for l in range(DEPTH):
        S.alias(arena_mix, B_xnf)
        for gi, (t0, nt) in enumerate(GROUPS):
            xv_, bx_ = xn_g[gi % 2], B_xng[gi % 2]
            rmsnorm_group(gi, V_GM + l * 8, xv_, bx_)
            wk, bwk = load_w(w_in_d[l, :, C_K:C_K + 512], KC, 512)
            for sub, ps, bps in proj(wk, bwk, xv_, bx_, nt):
                st, bst = tf.get()
                cp("act", st[:, :nt], ps[:, :nt], [bps], [bst])
                S.dma("sp", lambda e, st=st, sub=sub, t0=t0, nt=nt: e.dma_start(
                    out=o_k[l, sub * 128:(sub + 1) * 128, t0:t0 + nt], in_=st[:, :nt]), reads=[bst])
                if gi < 4:
                    S.dma("pool", lambda e, st=st, sub=sub, t0=t0, nt=nt: e.dma_start(
                        out=xk_d.ap()[sub * 128:(sub + 1) * 128, t0:t0 + nt], in_=st[:, :nt]),
                        reads=[bst], writes=[B_xk])
            wv, bwv = load_w(w_in_d[l, :, C_V:C_V + 512], KC, 512)
            for blk in range(nt // 128):
                ps, bps = psum()
                for kc in range(KC):
                    mm(ps[:, :], xv_[:, kc, blk * 128:(blk + 1) * 128], wv[:, kc, :], kc == 0, kc == KC - 1,
                       [bwv, bx_], bps)
                st, bst = tf.get()
                cp("dve", st[:, :], ps[:, :], [bps], [bst])
                r0 = t0 + blk * 128
                S.dma("sp", lambda e, st=st, r0=r0: e.dma_start(out=o_v[l, r0:r0 + 128, :], in_=st[:, :]),
                      reads=[bst])
                if gi < 4:
                    S.dma("pool", lambda e, st=st, r0=r0: e.dma_start(out=xv_d.ap()[r0:r0 + 128, :], in_=st[:, :]),
                          reads=[bst], writes=[B_xv])
            wc, bwc = load_w(w_in_d[l, :, C_C:C_C + 512], KC, 512)
            wh, bwh = load_w(w_in_d[l, :, C_H:C_H + 512], KC, 512)
            for sub in range(4):
                psc, bpc = psum()
                for kc in range(KC):
                    mm(psc[:, :nt], wc[:, kc, sub * 128:(sub + 1) * 128], xv_[:, kc, :nt], kc == 0, kc == KC - 1,
                       [bwc, bx_], bpc)
                psh, bph = psum()
                for kc in range(KC):
                    mm(psh[:, :nt], wh[:, kc, sub * 128:(sub + 1) * 128], xv_[:, kc, :nt], kc == 0, kc == KC - 1,
                       [bwh, bx_], bph)
                cs, bcs = tf.get()
                cp("act", cs[:, :nt], psc[:, :nt], [bpc], [bcs])
                uf, buf_ = tf.get()
                tt("dve", uf[:, :nt], cs[:, :nt], psh[:, :nt], ALU.mult, [bcs, bph], [buf_])
                if gi < 4:
                    j0 = gi * 4
                    ufv = uf[:, :].rearrange("p (j t) -> p j t", j=4)
                    cp("pool", uT[:, sub, j0:j0 + 4, 2:130], ufv, [buf_], [B_uT])
                    cp("pool", tails[:, sub, j0:j0 + 4, :], ufv[:, :, 126:128], [buf_], [B_tails])
                else:
                    cp("pool", full[:, sub, 2:6, :], uf[:, 0:nt].rearrange("p (t b) -> p t b", t=4),
                       [buf_], [B_full])
        S.dma("sp", lambda e: e.dma_start(out=full[:, :, 0:2, :], in_=scT_d[l]), writes=[B_full])
        S.dma("sp", lambda e: e.dma_start(out=o_convs[l], in_=full[:, :, 4:6, :]), reads=[B_full])
        S.dma("sp", lambda e: e.dma_start(out=o_tail[l], in_=tails[:]), reads=[B_tails])
        S.dma("sp", lambda e: e.dma_start(out=xt_d.ap().rearrange("(s p) c -> p s c", p=128),
                                          in_=tails[:].rearrange("p s j i -> p s (j i)")),
              reads=[B_tails], writes=[B_xt])
        if (stop_after == "pass1" and l == 0) or (stop_after == "l1pass1" and l == 1):
            break
        S.op("pool", lambda e: e.collective_compute("AllGather", ALU.bypass, replica_groups=pairs,
                                                    ins=[xk_d.ap()], outs=[gk_d.ap()]), reads=[B_xk], writes=[B_gk])
        S.op("pool", lambda e: e.collective_compute("AllGather", ALU.bypass, replica_groups=pairs,
                                                    ins=[xv_d.ap()], outs=[gv_d.ap()]), reads=[B_xv], writes=[B_gv])
        S.op("pool", lambda e: e.collective_compute("AllGather", ALU.bypass, replica_groups=pairs,
                                                    ins=[xt_d.ap()], outs=[gt_d.ap()]), reads=[B_xt], writes=[B_gt])
        S.dma("sp", lambda e: e.dma_start(out=th[:].rearrange("p r s j i -> p (r s) (j i)"),
                                          in_=gt_d.ap().rearrange("(rs p) c -> p rs c", p=128)),
              reads=[B_gt], writes=[B_th])
        for sub in range(4):
            hl, bh = tf.get()
            hv = hl[:, 0:32].rearrange("p (j i) -> p j i", j=NPB)
            S.op("dve", lambda e, hl=hl: e.memset(hl[:, 0:32], 0.0), writes=[bh])
            ts("dve", hv[:, 1:NPB, :], th[:, 1, sub, 0:NPB - 1, :], vcol(V_SEL), ALU.mult, [B_th, B_vec], [bh])
            stt("dve", uT[:, sub, :, 0:2], th[:, 0, sub, :, :], vcol(V_SEL + 1), hv, ALU.mult, ALU.add,
                [B_th, B_vec, bh], [B_uT])

        gkv = gk_d.ap().rearrange("(r hp p) (j t) -> r hp p j t", r=2, hp=4, t=128)
        gvv = gv_d.ap().rearrange("(r j t) (hp c) -> r j t hp c", r=2, t=128, hp=4)

        def attention(G):
            for hp in range(4):
                Zb = [[(PS[0], B_PS[0]), (PS[1], B_PS[1])], [(PS[2], B_PS[2]), (PS[3], B_PS[3])]]
                Xb = [(PS[4], B_PS[4]), (PS[5], B_PS[5])]
                Ob = (PS[6], B_PS[6])
                for hh in range(2):
                    mm(Xb[hh][0][:, :], zeros[:, 0:128], zeros[:, :], True, False, [B_zeros], Xb[hh][1])
                mm(Ob[0][:, :], zeros[:, 0:128], zeros[:, :], True, False, [B_zeros], Ob[1])
                gl = list(range(8 * G + 7, -1, -1))

                def geom(g):
                    m = g - 8 * G
                    return m, (0 if m < 0 else (m // 2) * 128)

                def load_blk(g):
                    kt, bkt = kvring.get()
                    r, j = g % 2, g // 2
                    S.dma("sp", lambda e: [
                        e.dma_start(out=kt[:, 0, :], in_=gkv[r, hp, :, j, :]),
                        e.dma_start(out=kt[:, 1, 0:64], in_=gvv[r, j, :, hp, 0:64]),
                        e.dma_start(out=kt[:, 2, 64:128], in_=gvv[r, j, :, hp, 64:128])],
                        reads=[B_gk, B_gv], writes=[bkt])
                    return kt, bkt

                def zmat(g, kt, bkt, par):
                    m, c0 = geom(g)
                    for hh in range(2):
                        rows = slice(64 * hh, 64 * hh + 64)
                        Z, bz = Zb[par][hh]
                        mm(Z[:, c0:512], kt[rows, 0, :], QT_g[rows, hp, c0:512], True, True, [bkt, B_QT], bz)

                blks = {}
                blks[gl[0]] = load_blk(gl[0])
                blks[gl[1]] = load_blk(gl[1])
                zmat(gl[0], blks[gl[0]][0], blks[gl[0]][1], 0)
                for idx, g in enumerate(gl):
                    par = idx % 2
                    kt, bkt = blks.pop(g)
                    m, c0 = geom(g)
                    head = [2 * hp, 2 * hp + 1]
                    E, SPt, Ft, Wt = [None, None], [None, None], [None, None], [None, None]
                    for hh in range(2):
                        Z, bz = Zb[par][hh]
                        E[hh] = tf.get()
                        act(E[hh][0][:, c0:512], Z[:, c0:512], AF.Exp, [bz, B_vec], [E[hh][1]],
                            bias=vcol(V_SBB + l * 8 + head[hh]), scale=0.125)
                    if idx + 2 < len(gl):
                        blks[gl[idx + 2]] = load_blk(gl[idx + 2])
                    if idx + 1 < len(gl):
                        gn = gl[idx + 1]
                        zmat(gn, blks[gn][0], blks[gn][1], 1 - par)
                    for hh in range(2):
                        if m >= 0:
                            tt("dve", E[hh][0][:, c0:512], E[hh][0][:, c0:512], masks[:, m, c0:512], ALU.mult,
                               [B_masks], [E[hh][1]])
                        SPt[hh] = tb.get()
                        act(SPt[hh][0][:, c0:512], E[hh][0][:, c0:512], AF.Ln, [E[hh][1]], [SPt[hh][1]], bias=1.0)
                    for hh in range(2):
                        mm(Xb[hh][0][:, c0:512], tri[:, 0, :], SPt[hh][0][:, c0:512], False, False,
                           [SPt[hh][1], B_tri], Xb[hh][1])
                    for hh in range(2):
                        Ft[hh] = tf.get()
                        act(Ft[hh][0][:, c0:512], Xb[hh][0][:, c0:512], AF.Exp, [Xb[hh][1]], [Ft[hh][1]], scale=-1.0)
                    for hh in range(2):
                        mm(Xb[hh][0][:, c0:512], tri[:, 1, :], SPt[hh][0][:, c0:512], False, False,
                           [SPt[hh][1], B_tri], Xb[hh][1])
                    for hh in range(2):
                        Wt[hh] = tb.get()
                        tt("dve", Wt[hh][0][:, c0:512], E[hh][0][:, c0:512], Ft[hh][0][:, c0:512], ALU.mult,
                           [E[hh][1], Ft[hh][1]], [Wt[hh][1]])
                    for hh in range(2):
                        mm(Ob[0][:, c0:512], kt[:, 1 + hh, :], Wt[hh][0][:, c0:512], False, False,
                           [Wt[hh][1], bkt], Ob[1])
                cp("act", attnT[:, hp, :], Ob[0][:, :], [Ob[1]], [B_attnT])

        def mixer_tail(gi, xv_, bx_, nt, conv_fn):
            t0 = GROUPS[gi][0]
            wB, bwB = load_w(w_in_d[l, :, C_B:C_B + 512], KC, 512)
            for sub, ps, bps in proj(wB, bwB, xv_, bx_, nt):
                cvt, bcv = conv_fn(sub)
                tt("dve", bcT[:, sub, :nt], cvt[:, :nt], ps[:, :nt], ALU.mult, [bcv, bps], [B_bcT])
            for half in range(2):
                c1 = half * 512
                wa, bwa = load_w(w_a_d[l, :, c1:c1 + 512], 4, 512)
                wga, bwga = load_w(w_in_d[l, :, C_GA + c1:C_GA + c1 + 512], KC, 512)
                for sub in range(4):
                    cs = slice(sub * 128, (sub + 1) * 128)
                    pya, bya = psum()
                    for kc in range(4):
                        mm(pya[:, :nt], wa[:, kc, cs], attnT[:, kc, :nt], kc == 0, kc == 3, [bwa, B_attnT], bya)
                    pga, bga = psum()
                    for kc in range(KC):
                        mm(pga[:, :nt], wga[:, kc, cs], xv_[:, kc, :nt], kc == 0, kc == KC - 1, [bwga, bx_], bga)
                    sa, bsa = tf.get()
                    act(sa[:, :nt], pga[:, :nt], AF.Sigmoid, [bga], [bsa])
                    tt("dve", mT[:, half * 4 + sub, :nt], sa[:, :nt], pya[:, :nt], ALU.mult, [bsa, bya], [B_mT])
                wb_, bwb = load_w(w_b_d[l, :, c1:c1 + 512], 4, 512)
                wgb, bwgb = load_w(w_in_d[l, :, C_GB + c1:C_GB + c1 + 512], KC, 512)
                for sub in range(4):
                    cs = slice(sub * 128, (sub + 1) * 128)
                    pyb, byb = psum()
                    for kc in range(4):
                        mm(pyb[:, :nt], wb_[:, kc, cs], bcT[:, kc, :nt], kc == 0, kc == 3, [bwb, B_bcT], byb)
                    pgb, bgb = psum()
                    for kc in range(KC):
                        mm(pgb[:, :nt], wgb[:, kc, cs], xv_[:, kc, :nt], kc == 0, kc == KC - 1, [bwgb, bx_], bgb)
                    sg, bsg = tf.get()
                    act(sg[:, :nt], pgb[:, :nt], AF.Sigmoid, [bgb], [bsg])
                    tt("dve", sg[:, :nt], sg[:, :nt], pyb[:, :nt], ALU.mult, [byb], [bsg])
                    tt("pool", mT[:, half * 4 + sub, :nt], mT[:, half * 4 + sub, :nt], sg[:, :nt], ALU.add,
                       [bsg], [B_mT])
            for half in range(2):
                c1 = half * 512
                wo, bwo = load_w(w_o_d[l, :, c1:c1 + 512], KC, 512)
                for sub, ps, bps in proj(wo, bwo, mT, B_mT, nt):
                    oc = half * 4 + sub
                    tt("dve", x[:, oc, t0:t0 + nt], x[:, oc, t0:t0 + nt], ps[:, :nt], ALU.add, [bps],
                       [B_x[oc][gi]])

        def conv_prompt(gi):
            def fn(sub):
                j0 = gi * 4
                c1t, b1 = tf.get()
                v1 = c1t[:, :].rearrange("p (j t) -> p j t", j=4)
                ts("pool", v1, uT[:, sub, j0:j0 + 4, 0:128], vcol(V_CW + l * 12 + 0 * 4 + sub), ALU.mult,
                   [B_uT, B_vec], [b1])
                stt("dve", v1, uT[:, sub, j0:j0 + 4, 1:129], vcol(V_CW + l * 12 + 1 * 4 + sub), v1, ALU.mult,
                    ALU.add, [B_uT, B_vec], [b1])
                stt("dve", v1, uT[:, sub, j0:j0 + 4, 2:130], vcol(V_CW + l * 12 + 2 * 4 + sub), v1, ALU.mult,
                    ALU.add, [B_uT, B_vec], [b1])
                return c1t, b1
            return fn

        def conv_sample(sub):
            c1t, b1 = tf.get()
            v1 = c1t[:, 0:128].rearrange("p (t b) -> p t b", t=4)
            ts("pool", v1, full[:, sub, 0:4, :], vcol(V_CW + l * 12 + 0 * 4 + sub), ALU.mult, [B_full, B_vec], [b1])
            stt("dve", v1, full[:, sub, 1:5, :], vcol(V_CW + l * 12 + 1 * 4 + sub), v1, ALU.mult, ALU.add,
                [B_full, B_vec], [b1])
            stt("dve", v1, full[:, sub, 2:6, :], vcol(V_CW + l * 12 + 2 * 4 + sub), v1, ALU.mult, ALU.add,
                [B_full, B_vec], [b1])
            return c1t, b1

        for gi in range(4):
            t0, nt = GROUPS[gi]
            xv_, bx_ = xn_g[gi % 2], B_xng[gi % 2]
            rmsnorm_group(gi, V_GM + l * 8, xv_, bx_)
            wq, bwq = load_w(w_in_d[l, :, C_Q:C_Q + 512], KC, 512)
            for sub, ps, bps in proj(wq, bwq, xv_, bx_, nt):
                cp("act", QT_g[:, sub, :], ps[:, :], [bps], [B_QT])
            attention(gi)
            mixer_tail(gi, xv_, bx_, nt, conv_prompt(gi))
        if stop_after == "prompt_mixer" and l == 0:
            break

        gi = 4
        t0, nt = GROUPS[gi]
        xv_, bx_ = xn_g[1], B_xng[1]
        rmsnorm_group(gi, V_GM + l * 8, xv_, bx_)
        if skip_sample:
            S.op("dve", lambda e: e.memset(attnT[:, :, 0:NS], 0.0), writes=[B_attnT])
        else:
            S.dma("pool", lambda e: e.dma_start(out=wqkv[:], in_=wqkv_d[l].rearrange("(k p) n -> p k n", p=128)),
                  writes=[B_wqkv])
            ps, bps = psum()
            for kc in range(KC):
                mm(ps[:, 0:64], xv_[:, kc, :NS], wqkv[:, kc, 0:64], kc == 0, kc == KC - 1, [B_wqkv, bx_], bps)
            cp("act", qtm[:, :], ps[:, 0:64], [bps], [B_qtm])
            S.dma("sp", lambda e: e.dma_start(out=qsd_d.ap(), in_=qtm[:, :]), reads=[B_qtm], writes=[B_qsd])
            ps, bps = psum()
            for kc in range(KC):
                mm(ps[0:64, 0:NS], wqkv[:, kc, 0:64], xv_[:, kc, :NS], kc == 0, kc == KC - 1, [B_wqkv, bx_], bps)
            cp("act", qsT[:, :], ps[0:64, 0:NS], [bps], [B_qs])
            ps, bps = psum()
            for kc in range(KC):
                mm(ps[0:64, 0:NS], wqkv[:, kc, 64:128], xv_[:, kc, :NS], kc == 0, kc == KC - 1, [B_wqkv, bx_], bps)
            cp("act", ksT[:, :], ps[0:64, 0:NS], [bps], [B_ks])
            ps, bps = psum()
            for kc in range(KC):
                mm(ps[:, 0:64], xv_[:, kc, :NS], wqkv[:, kc, 128:192], kc == 0, kc == KC - 1, [B_wqkv, bx_], bps)
            cp("act", vnt[:, :], ps[:, 0:64], [bps], [B_vn])
            bias_c = vcol(V_SBBC + l)
            OPS, B_OPS = PS[7], B_PS[7]
            pzn, bzn = psum()
            mm(pzn[:, 0:NS], ksT[:, :], qsT[:, :], True, True, [B_qs, B_ks], bzn)
            en, ben = tf.get()
            act(en[:, 0:NS], pzn[:, 0:NS], AF.Exp, [bzn, B_vec], [ben], bias=bias_c, scale=0.125)
            tt("dve", en[:, 0:NS], en[:, 0:NS], maskn2, ALU.mult, [B_const], [ben])
            spn, bspn = tb.get()
            act(spn[:, 0:NS], en[:, 0:NS], AF.Ln, [ben], [bspn], bias=1.0)
            pcn, bcn = psum()
            mm(pcn[:, 0:NS], tri[:, 3, :], spn[:, 0:NS], True, True, [bspn, B_tri], bcn)
            ptn, btn = psum()
            mm(ptn[:, 0:NS], tri[:, 2, :], spn[:, 0:NS], True, True, [bspn, B_tri], btn)
            cp("dve", tnt[:, :], ptn[:, 0:NS], [btn], [B_tn])
            fn_, bfn = tf.get()
            act(fn_[:, 0:NS], pcn[:, 0:NS], AF.Exp, [bcn], [bfn], scale=-1.0)
            wn, bwn = tf.get()
            tt("dve", wn[:, 0:NS], en[:, 0:NS], fn_[:, 0:NS], ALU.mult, [ben, bfn], [bwn])
            mm(OPS[0:64, NS:2 * NS], vnt[:, :], wn[:, 0:NS], True, True, [B_vn, bwn], B_OPS)
            mm(OPS[0:64, 0:NS], zeros[:, 0:64], zeros[:, 0:NS], True, False, [B_zeros], B_OPS)
            tnv = tnt[:, :].rearrange("p (t b) -> p t b", t=4)
            qsdv = qsd_d.ap().rearrange("(t b) d -> t b d", t=4)
            gk_f = arena[:, 0:4096].bitcast(F32)
            gkv_ = gk_f.rearrange("p (k d) -> p k d", k=32)
            pr_f = arena[:, 14336:18432].bitcast(F32)
            prv = pr_f.rearrange("p (k d) -> p k d", k=32)
            for pair in range(DB // 2):
                b0 = 2 * pair
                S.dma("sp", lambda e: [e.dma_start(out=qrep[0:64, :, :], in_=qsdv[:, b0, :].partition_broadcast(64)),
                                       e.dma_start(out=qrep[64:128, :, :],
                                                   in_=qsdv[:, b0 + 1, :].partition_broadcast(64))],
                      reads=[B_qsd], writes=[B_qrep])
                Zt, bZ = tf.get()
                Ztv = Zt[:, :].rearrange("p (k t) -> p k t", t=4)
                gvs = []
                for qd in range(4):
                    ti = l * 4 + qd
                    S.dma("pool", lambda e, ti=ti: e.indirect_dma_start(
                        out=gk_f, out_offset=None, in_=ckq_d[ti],
                        in_offset=bass.IndirectOffsetOnAxis(ap=ptT2[:, pair:pair + 1], axis=0)),
                        reads=[B_pt], writes=[B_xng[0]])
                    vtt, bvt = wring.get()
                    vf = vtt[:, :].bitcast(F32)
                    S.dma("pool", lambda e, ti=ti, vf=vf: e.indirect_dma_start(
                        out=vf, out_offset=None, in_=cvq_d[ti],
                        in_offset=bass.IndirectOffsetOnAxis(ap=ptT2[:, pair:pair + 1], axis=0)),
                        reads=[B_pt], writes=[bvt])
                    gvs.append((vf.rearrange("p (k d) -> p k d", k=32), bvt))
                    for t in range(4):
                        tt("dve", prv, gkv_, qrep[:, t:t + 1, :].to_broadcast([128, 32, 64]), ALU.mult,
                           [B_xng[0], B_qrep], [B_mT])
                        S.op("dve", lambda e, qd=qd, t=t: e.tensor_reduce(
                            out=Ztv[:, qd * 32:(qd + 1) * 32, t], in_=prv, axis=AX.X, op=ALU.add),
                            reads=[B_mT], writes=[bZ])
                if DBG and l == 0 and pair == 0:
                    S.dma("sp", lambda e, Zt=Zt: e.dma_start(out=d_z, in_=Zt[:, :]), reads=[bZ])
                E_, bE = tf.get()
                act(E_[:, :], Zt[:, :], AF.Exp, [bZ, B_vec], [bE], bias=bias_c, scale=0.125)
                SPf, bSP = tf.get()
                act(SPf[:, :], E_[:, :], AF.Ln, [bE], [bSP], bias=1.0)
                SPv = SPf[:, :].rearrange("p (k t) -> p t k", t=4)
                S.op("dve", lambda e, SPv=SPv: e.tensor_reduce(out=tot4[:, 0:4], in_=SPv, axis=AX.X, op=ALU.add),
                     reads=[bSP], writes=[B_tot4])
                pcs, bcs_ = psum()
                mm(pcs[:, 0:4], u2[:, :], tot4[:, 0:4], True, True, [B_tot4, B_const], bcs_)
                cp("dve", gtn[0:64, 0:4], tnv[0:64, :, b0], [B_tn], [B_gtn])
                cp("dve", gtn[64:128, 0:4], tnv[64:128, :, b0 + 1], [B_tn], [B_gtn])
                tt("dve", gtn[:, 0:4], gtn[:, 0:4], tot4[:, 0:4], ALU.add, [B_tot4], [B_gtn])
                stt("dve", gtn[:, 0:4], gtn[:, 0:4], -1.0, pcs[:, 0:4], ALU.mult, ALU.subtract, [bcs_], [B_gtn])
                P_, bP = tf.get()
                for t in range(4):
                    S.op("dve", lambda e, t=t, P_=P_, SPf=SPf: e.tensor_tensor_scan(
                        out=P_[:, t:512:4], data0=onesf[:, 0:128], data1=SPf[:, t:512:4], initial=gtn[:, t:t + 1],
                        op0=ALU.mult, op1=ALU.add), reads=[bSP, B_gtn, B_zeros], writes=[bP])
                tt("dve", P_[:, :], SPf[:, :], P_[:, :], ALU.subtract, [bSP], [bP])
                if DBG and l == 0 and pair == 0:
                    S.dma("sp", lambda e, P_=P_: e.dma_start(out=d_x, in_=P_[:, :]), reads=[bP])
                    S.dma("sp", lambda e: [e.dma_start(out=d_g[:, 0:4], in_=gtn[:, 0:4]),
                                           e.dma_start(out=d_g[:, 4:8], in_=tot4[:, 0:4])], reads=[B_gtn, B_tot4])
                    S.dma("sp", lambda e: e.dma_start(out=d_tn, in_=tnt[:, :]), reads=[B_tn])
                act(P_[:, :], P_[:, :], AF.Exp, [bP], [bP], scale=-1.0)
                wp = pr_f[:, 0:1024]
                wpv = wp.rearrange("p (k t j) -> p k t j", t=4, j=2)
                wp8 = wp.rearrange("p (k c) -> p k c", c=8)
                P3 = P_[:, :].rearrange("p (k t) -> p k t", t=4)
                E3 = E_[:, :].rearrange("p (k t) -> p k t", t=4)
                if DBG and l == 0 and pair == 0:
                    S.dma("sp", lambda e, P_=P_: e.dma_start(out=d_w, in_=P_[:, :]), reads=[bP])
                S.op("dve", lambda e, wp=wp: e.memset(wp, 0.0), writes=[B_mT])
                tt("dve", wpv[0:64, :, :, 0], P3[0:64, :, :], E3[0:64, :, :], ALU.mult, [bP, bE], [B_mT])
                tt("dve", wpv[64:128, :, :, 1], P3[64:128, :, :], E3[64:128, :, :], ALU.mult, [bP, bE], [B_mT])
                for kk in range(128):
                    gvv_, bvt = gvs[kk // 32]
                    mm(OPS[0:64, pair * 8:(pair + 1) * 8], gvv_[:, kk % 32, :], wp8[:, kk, :], False, False,
                       [bvt, B_mT], B_OPS)
            otmp, botmp = tf.get()
            cp("act", otmp[0:64, 0:NS].rearrange("p (t q j) -> p t q j", t=4, j=2),
               OPS[0:64, 0:NS].rearrange("p (q t j) -> p t q j", t=4, j=2), [B_OPS], [botmp])
            tt("dve", ospT[:, :], otmp[0:64, 0:NS], OPS[0:64, NS:2 * NS], ALU.add, [botmp, B_OPS], [B_osp])
            if DBG and l == 0:
                S.dma("sp", lambda e, otmp=otmp: e.dma_start(out=d_osp, in_=otmp[0:64, 0:NS]), reads=[botmp])
                o2, bo2 = tf.get()
                cp("act", o2[0:64, 0:NS], OPS[0:64, NS:2 * NS], [B_OPS], [bo2])
                S.dma("sp", lambda e, o2=o2: e.dma_start(out=d_new, in_=o2[0:64, 0:NS]), reads=[bo2])
            S.dma("sp", lambda e: e.dma_start(out=xs_d.ap(), in_=ospT[:, :]), reads=[B_osp], writes=[B_xs])
            S.op("pool", lambda e: e.collective_compute("AllGather", ALU.bypass, replica_groups=[list(range(8))],
                                                        ins=[xs_d.ap()], outs=[gs_d.ap()]),
                 reads=[B_xs], writes=[B_gs])
            S.op("pool", lambda e: e.collective_compute("AllGather", ALU.bypass, replica_groups=[list(range(8))],
                                                        ins=[xs_d.ap()], outs=[gs_d.ap()]),
                 reads=[B_xs], writes=[B_gs])
            S.dma("sp", lambda e: e.dma_start(out=attnT[:, :, 0:NS],
                                              in_=gs_d.ap().rearrange("(k p) n -> p k n", p=128)),
                  reads=[B_gs], writes=[B_attnT])
        mixer_tail(4, xv_, bx_, NS, conv_sample)
        if stop_after == "mixer" and l == 0:
            break

        S.alias(B_xnf, arena_mix)
        for gi, (t0, nt) in enumerate(GROUPS):
            rstd, br = rstd_group(gi, keep=True)
            for kc in range(KC):
                stt("dve", xn_full[:, kc, t0:t0 + nt], x[:, kc, t0:t0 + nt], vcol(V_GF + l * 8 + kc), rstd[:, :nt],
                    ALU.mult, ALU.mult, [B_x[kc][gi], br, B_vec], [B_xnf[gi]])
            if l % 2 == 1:
                for blk in range(nt // 128):
                    c0 = blk * 128
                    pl, bl_ = psum()
                    for kc in range(KC):
                        xf, bxf = tf.get()
                        stt("dve", xf[:, 0:128], x[:, kc, t0 + c0:t0 + c0 + 128], vcol(V_GF + l * 8 + kc),
                            rstd[:, c0:c0 + 128], ALU.mult, ALU.mult, [B_x[kc][gi], br, B_vec], [bxf])
                        mm(pl[:, 0:NE], xf[:, 0:128], rt[:, kc, :], kc == 0, kc == KC - 1, [bxf, B_const], bl_)
                    jb = (t0 + c0) // 128
                    w1, bw1 = tf.get()
                    lg = w1[:, 0:8]
                    m1 = w1[:, 8:9]
                    eq1 = w1[:, 16:24]
                    lg2 = w1[:, 24:32]
                    m2 = w1[:, 32:33]
                    eq2 = w1[:, 40:48]
                    dd = w1[:, 48:49]
                    g1 = w1[:, 49:50]
                    g2 = w1[:, 50:51]
                    cp("dve", lg, pl[:, 0:NE], [bl_], [bw1])
                    S.op("dve", lambda e, m1=m1, lg=lg: e.tensor_reduce(out=m1, in_=lg, axis=AX.X, op=ALU.max),
                         writes=[bw1])
                    ts("dve", eq1, lg, m1, ALU.is_equal, [], [bw1])
                    stt("dve", lg2, eq1, -1e30, lg, ALU.mult, ALU.add, [], [bw1])
                    S.op("dve", lambda e, m2=m2, lg2=lg2: e.tensor_reduce(out=m2, in_=lg2, axis=AX.X, op=ALU.max),
                         writes=[bw1])
                    ts("dve", eq2, lg2, m2, ALU.is_equal, [], [bw1])
                    tt("dve", dd, m2, m1, ALU.subtract, [], [bw1])
                    act(dd, dd, AF.Exp, [bw1], [bw1])
                    ts("dve", g1, dd, 1.0, ALU.add, [], [bw1])
                    S.op("dve", lambda e, g1=g1: e.reciprocal(out=g1, in_=g1), writes=[bw1])
                    tt("dve", g2, dd, g1, ALU.mult, [], [bw1])
                    ts("dve", eq1, eq1, g1, ALU.mult, [], [bw1])
                    stt("dve", comb[:, jb, :], eq2, g2, eq1, ALU.mult, ALU.add, [bw1], [B_comb])

        def ffn_pass(wg_ap, wu_ap, wd_ap, dff, use_comb, e_idx):
            if use_comb:
                for jb0 in range(0, NT // 128, 4):
                    pcb, bcb = psum()
                    nb_ = min(4, NT // 128 - jb0)
                    for q in range(nb_):
                        dg, bdg = tb.get()
                        ts("dve", dg[:, 0:128], ident, comb[:, jb0 + q, e_idx:e_idx + 1], ALU.mult,
                           [B_comb, B_const], [bdg])
                        mm(pcb[:, q * 128:(q + 1) * 128], tri[:, 2, :], dg[:, 0:128], True, True, [bdg, B_tri], bcb)
                    cp("act", comb_b[:, jb0 * 128:(jb0 + nb_) * 128], pcb[:, 0:nb_ * 128], [bcb], [B_combb])
            nhg = (dff + 511) // 512
            for hg in range(nhg):
                h0 = hg * 512
                hn = min(512, dff - h0)
                nsub = hn // 128
                wg, bwg = load_w(wg_ap[:, h0:h0 + hn], KC, hn)
                wu, bwu = load_w(wu_ap[:, h0:h0 + hn], KC, hn)
                wd, bwd = load_w(wd_ap[h0:h0 + hn, :], nsub, D)
                for gi, (t0, nt) in enumerate(GROUPS):
                    hs = []
                    for sub in range(nsub):
                        cs = slice(sub * 128, (sub + 1) * 128)
                        pg, bpg = psum()
                        for kc in range(KC):
                            mm(pg[:, :nt], wg[:, kc, cs], xn_full[:, kc, t0:t0 + nt], kc == 0, kc == KC - 1,
                               [bwg, B_xnf[gi]], bpg)
                        pu, bpu = psum()
                        for kc in range(KC):
                            mm(pu[:, :nt], wu[:, kc, cs], xn_full[:, kc, t0:t0 + nt], kc == 0, kc == KC - 1,
                               [bwu, B_xnf[gi]], bpu)
                        sg, bsg = tf.get()
                        act(sg[:, :nt], pg[:, :nt], AF.Silu, [bpg], [bsg])
                        ht, bht = tb.get()
                        if use_comb:
                            tt("dve", sg[:, :nt], sg[:, :nt], pu[:, :nt], ALU.mult, [bpu], [bsg])
                            tt("pool", ht[:, :nt], sg[:, :nt], comb_b[:, t0:t0 + nt], ALU.mult, [bsg, B_combb], [bht])
                        else:
                            tt("dve", ht[:, :nt], sg[:, :nt], pu[:, :nt], ALU.mult, [bsg, bpu], [bht])
                        hs.append((ht, bht))
                    for oc in range(KC):
                        ps, bps = psum()
                        for sub in range(nsub):
                            mm(ps[:, :nt], wd[:, sub, oc * 128:(oc + 1) * 128], hs[sub][0][:, :nt], sub == 0,
                               sub == nsub - 1, [bwd, hs[sub][1]], bps)
                        tt("dve", x[:, oc, t0:t0 + nt], x[:, oc, t0:t0 + nt], ps[:, :nt], ALU.add, [bps],
                           [B_x[oc][gi]])

        if l % 2 == 0:
            ffn_pass(fwg_d[l // 2], fwu_d[l // 2], fwd_d[l // 2], D_FF, False, 0)
        else:
            for e_idx in range(NE):
                ffn_pass(mwg_d[l // 2, e_idx], mwu_d[l // 2, e_idx], mwd_d[l // 2, e_idx], D_FFE, True, e_idx)
        if stop_after == "layer0" and l == 0:
            break

    for gi, (t0, nt) in enumerate(GROUPS):
        rstd, br = rstd_group(gi, keep=True)
        for kc in range(KC):
            yo, byo = tf.get()
            stt("dve", yo[:, :nt], x[:, kc, t0:t0 + nt], vcol(V_GFIN + kc), rstd[:, :nt],
                ALU.mult, ALU.mult, [B_x[kc][gi], br, B_vec], [byo])
            S.dma("sp", lambda e, yo=yo, kc=kc, t0=t0, nt=nt: e.dma_start(
                out=o_y[kc * 128:(kc + 1) * 128, t0:t0 + nt], in_=yo[:, :nt]), reads=[byo])
    S.finish()
    print("instr/waits:", S.stats())
    nc._declared_inputs = declared
    return nc


_PROGRAM_CACHE = {}


def _consts():
    p = np.arange(128)
    cst = np.zeros((128, 7, 128), np.float32)
    cst[:, 0, :] = (p[:, None] >= p[None, :])
    cst[:, 1, :] = (p[:, None] < p[None, :])
    cst[:, 2, :] = 1.0
    t_ = p // DB
    b_ = p % DB
    same = b_[:, None] == b_[None, :]
    cst[:, 3, :] = same & (t_[:, None] >= t_[None, :])
    cst[:, 4, :] = same & (t_[:, None] < t_[None, :])
    cst[:, 5, :] = np.eye(128)
    cst[:, 6, :] = ((p[:, None] // 64) == (p[None, :] // 64)) & (p[:, None] > p[None, :])
    return cst


def _masks(r):
    p = np.arange(128)[:, None, None, None]
    m = np.arange(8)[None, :, None, None]
    jj = np.arange(4)[None, None, :, None]
    c = np.arange(128)[None, None, None, :]
    mk = ((m - 2 * jj - r) * 128 + p - c) < 0
    return np.ascontiguousarray(mk.reshape(128, 8, 512).astype(np.float32))


def _prep_inputs(inp):
    f = lambda a: np.ascontiguousarray(np.asarray(a, dtype=np.float32))
    x_prompt, x_sample = f(inp["x_prompt"]), f(inp["x_sample"])
    ckq_all = np.ascontiguousarray(np.asarray(inp["cache_k"], dtype=np.float32).reshape(
        DEPTH, N_POOL, 4, 32, 8, 64).transpose(4, 0, 2, 1, 3, 5))
    cvq_all = np.ascontiguousarray(np.asarray(inp["cache_v"], dtype=np.float32).reshape(
        DEPTH, N_POOL, 4, 32, 8, 64).transpose(4, 0, 2, 1, 3, 5))
    w_in = f(inp["w_in"])
    shared = {
        "w_in": w_in, "w_a": f(inp["w_a"]), "w_b": f(inp["w_b"]), "w_o": f(inp["w_o"]),
        "ffn_wg": f(inp["ffn_wg"]), "ffn_wu": f(inp["ffn_wu"]), "ffn_wd": f(inp["ffn_wd"]),
        "moe_wg": f(inp["moe_wg"]), "moe_wu": f(inp["moe_wu"]), "moe_wd": f(inp["moe_wd"]),
        "router": np.ascontiguousarray(f(inp["router"])[0].reshape(KC, 128, NE).transpose(1, 0, 2)),
        "cst": _consts(),
        "ptT2": np.ascontiguousarray(np.asarray(inp["page_table"], dtype=np.int32).reshape(DB // 2, 128).T),
        "scT": np.ascontiguousarray(f(inp["state_conv"]).reshape(DEPTH, DB, 2, 4, 128).transpose(0, 4, 3, 2, 1)),
    }
    xs_T = np.ascontiguousarray(x_sample.transpose(1, 0, 2).reshape(NS, D).T)
    nm, nf, nfin = f(inp["norm_mix"]), f(inp["norm_ffn"]), f(inp["norm_final"])
    cw, sbb = f(inp["conv_w"]), f(inp["sb_bias"])
    in_maps = []
    for c in range(8):
        b, r = c // 2, c % 2
        xp = x_prompt[b].reshape(32, 128, D)[r::2].reshape(NP, D).T
        xT = np.ascontiguousarray(np.concatenate([xp, xs_T], axis=1))
        vec = np.zeros((128, NVEC), np.float32)
        for l in range(DEPTH):
            vec[:, V_GM + l * 8:V_GM + (l + 1) * 8] = nm[l].reshape(KC, 128).T
            vec[:, V_GF + l * 8:V_GF + (l + 1) * 8] = nf[l].reshape(KC, 128).T
            for k in range(3):
                vec[:, V_CW + l * 12 + k * 4:V_CW + l * 12 + (k + 1) * 4] = cw[l, k].reshape(4, 128).T
            vec[:, V_SBB + l * 8:V_SBB + (l + 1) * 8] = sbb[l][None, :]
            vec[:, V_SBBC + l] = sbb[l, c]
        vec[:, V_GFIN:V_GFIN + 8] = nfin.reshape(KC, 128).T
        vec[:, V_SEL] = 1.0 if r == 0 else 0.0
        vec[:, V_SEL + 1] = 0.0 if r == 0 else 1.0
        vec[:, V_EPS] = EPS
        hs = slice(c * 64, (c + 1) * 64)
        wqkv = np.ascontiguousarray(np.concatenate(
            [w_in[:, :, C_Q + c * 64:C_Q + (c + 1) * 64], w_in[:, :, C_K + c * 64:C_K + (c + 1) * 64],
             w_in[:, :, C_V + c * 64:C_V + (c + 1) * 64]], axis=2))
        m = dict(shared)
        m.update({
            "xT": xT, "vec": vec, "masks": _masks(r), "wqkv": wqkv,

        })
        for i in range(DEPTH * 4):
            m["ckq%d" % i] = ckq_all[c].reshape(DEPTH * 4, N_POOL, 2048)[i]
            m["cvq%d" % i] = cvq_all[c].reshape(DEPTH * 4, N_POOL, 2048)[i]
        in_maps.append(m)
    return in_maps


def _assemble(results):
    B, SEQ = 4, 4096
    y_prompt = np.zeros((B, SEQ, D), np.float32)
    new_k_prompt = np.zeros((DEPTH, B, SEQ, 8, 64), np.float32)
    new_v_prompt = np.zeros((DEPTH, B, SEQ, 8, 64), np.float32)
    new_conv_prompt = np.zeros((DEPTH, B, 2, 512), np.float32)
    for c in range(8):
        b, r = c // 2, c % 2
        res = results[c]
        oy = np.asarray(res["o_y"])
        y_prompt[b].reshape(32, 128, D)[r::2] = oy[:, :NP].T.reshape(16, 128, D)
        ok = np.asarray(res["o_k"])
        ov = np.asarray(res["o_v"])
        for l in range(DEPTH):
            new_k_prompt[l, b].reshape(32, 128, 512)[r::2] = ok[l][:, :NP].T.reshape(16, 128, 512)
            new_v_prompt[l, b].reshape(32, 128, 512)[r::2] = ov[l][:NP].reshape(16, 128, 512)
        if r == 1:
            ot = np.asarray(res["o_tail"])
            for l in range(DEPTH):
                new_conv_prompt[l, b] = ot[l][:, :, NPB - 1, :].transpose(2, 1, 0).reshape(2, 512)
    r0 = results[0]
    oy = np.asarray(r0["o_y"])
    y_sample = np.ascontiguousarray(oy[:, NP:].T.reshape(4, DB, D).transpose(1, 0, 2))
    ok = np.asarray(r0["o_k"])
    ov = np.asarray(r0["o_v"])
    new_k_sample = np.stack([ok[l][:, NP:].T.reshape(4, DB, 8, 64).transpose(1, 0, 2, 3) for l in range(DEPTH)])
    new_v_sample = np.stack([ov[l][NP:].reshape(4, DB, 8, 64).transpose(1, 0, 2, 3) for l in range(DEPTH)])
    oc = np.asarray(r0["o_convs"])
    new_conv_sample = np.ascontiguousarray(oc.transpose(0, 4, 3, 2, 1).reshape(DEPTH, DB, 2, 512))
    return (y_prompt, y_sample, np.ascontiguousarray(new_k_prompt), np.ascontiguousarray(new_v_prompt),
            new_conv_prompt, np.ascontiguousarray(new_k_sample), np.ascontiguousarray(new_v_sample),
            new_conv_sample)


def kernel(**inputs):
    in_maps = _prep_inputs(inputs)
    nc = build_program()
    in_maps = [{k: m[k] for k in nc._declared_inputs} for m in in_maps]
    res = run_bass_kernel_spmd(nc, in_maps, core_ids=list(range(8)))
    return _assemble(res.results)
```
